# Optimizing a Trainium2 kernel written in Bass

```python
import math
import jax, jax.numpy as jnp
from jax import lax
import numpy as np

D_MODEL = 1024
BATCH = 32
SEQ = 2048
DEPTH = 4

N_A_LAYERS = DEPTH // 2
N_B_LAYERS = DEPTH - N_A_LAYERS
D_FF = 4 * D_MODEL

A_HEADS = 4
A_QK_DIM = D_MODEL // (2 * A_HEADS)
A_V_DIM = D_MODEL // A_HEADS
A_CHUNK = 64
A_Q_COLS = A_HEADS * A_QK_DIM
A_V_COLS = A_HEADS * A_V_DIM
A_IN_COLS = 2 * A_Q_COLS + 2 * A_V_COLS + 2 * A_HEADS

B_HEADS = 8
B_HEAD_DIM = D_MODEL // (2 * B_HEADS)
B_V_DIM = 2 * B_HEAD_DIM
B_Q_COLS = B_HEADS * 2 * B_HEAD_DIM
B_K_COLS = B_HEADS * 2 * B_HEAD_DIM
B_KV_COLS = B_K_COLS + B_HEADS * B_V_DIM
Q_BLOCK = 128
ROPE_THETA = 500000.0
ROPE_DIM = B_HEAD_DIM // 4

NORM_EPS = 1e-6
SUBLN_EPS = 1e-5

kernel_name = "yoco_mlstm_diffattn_hybrid"


def lambda_init(layer_number):
    return 0.8 - 0.6 * math.exp(-0.3 * (layer_number - 1))


def rms_norm(x, g, eps=NORM_EPS):
    xf = x.astype(jnp.float32)
    y = xf * lax.rsqrt(jnp.mean(jnp.square(xf), axis=-1, keepdims=True) + eps)
    return (y * g.astype(jnp.float32)).astype(x.dtype)


def modulate(y, shift, scale):
    return y * (1 + scale) + shift


def sq_relu_mlp(y, w_up, w_down):
    return jnp.square(jax.nn.relu(y @ w_up)) @ w_down


def rope_tables(positions):
    inv_freq = jnp.power(ROPE_THETA, -jnp.arange(0, ROPE_DIM, 2, dtype=jnp.float32) / ROPE_DIM)
    ang = positions.astype(jnp.float32)[..., None] * inv_freq
    return jnp.cos(ang), jnp.sin(ang)


def apply_partial_rope(t, cos, sin):
    shape = cos.shape[:2] + (1,) * (t.ndim - 3) + (cos.shape[-1],)
    cos = cos.reshape(shape).astype(t.dtype)
    sin = sin.reshape(shape).astype(t.dtype)
    half = ROPE_DIM // 2
    r1 = t[..., :half]
    r2 = t[..., half:ROPE_DIM]
    return jnp.concatenate([r1 * cos - r2 * sin, r2 * cos + r1 * sin, t[..., ROPE_DIM:]], axis=-1)


def mlstm_chunkwise(q, k, v, i_pre, log_f):
    B, S, H, DK = q.shape
    DV = v.shape[-1]
    L = A_CHUNK
    NC = S // L
    f32 = jnp.float32

    def chunk_vec(t):
        return t.astype(f32).reshape(B, NC, L, H, t.shape[-1]).transpose(1, 0, 3, 2, 4)

    def chunk_gate(t):
        return t.astype(f32).reshape(B, NC, L, H).transpose(1, 0, 3, 2)

    qc, kc, vc = chunk_vec(q), chunk_vec(k), chunk_vec(v)
    ic = chunk_gate(i_pre)
    gc = jnp.cumsum(chunk_gate(log_f), axis=-1)
    causal = jnp.tril(jnp.ones((L, L), dtype=bool))

    def step(carry, xs):
        C, n, m = carry
        qb, kb, vb, ib, gb = xs
        G = gb[..., -1]
        Dm = jnp.where(causal, gb[..., :, None] - gb[..., None, :] + ib[..., None, :], -jnp.inf)
        inter = gb + m[..., None]
        m_row = jnp.maximum(inter, jnp.max(Dm, axis=-1))
        w_intra = jnp.exp(Dm - m_row[..., None])
        w_inter = jnp.exp(inter - m_row)
        s = jnp.einsum('bhld,bhsd->bhls', qb, kb) * w_intra
        num = jnp.einsum('bhls,bhsv->bhlv', s, vb) + w_inter[..., None] * jnp.einsum('bhld,bhdv->bhlv', qb, C)
        den = jnp.sum(s, axis=-1) + w_inter * jnp.einsum('bhld,bhd->bhl', qb, n)
        h = num / jnp.maximum(jnp.abs(den), jnp.exp(-m_row))[..., None]
        a = G[..., None] - gb + ib
        m_new = jnp.maximum(G + m, jnp.max(a, axis=-1))
        w_old = jnp.exp(G + m - m_new)
        w_s = jnp.exp(a - m_new[..., None])
        C_new = w_old[..., None, None] * C + jnp.einsum('bhs,bhsd,bhsv->bhdv', w_s, kb, vb)
        n_new = w_old[..., None] * n + jnp.einsum('bhs,bhsd->bhd', w_s, kb)
        return (C_new, n_new, m_new), h

    init = (jnp.zeros((B, H, DK, DV), f32), jnp.zeros((B, H, DK), f32), jnp.zeros((B, H), f32))
    _, hs = lax.scan(step, init, (qc, kc, vc, ic, gc))
    return hs.transpose(1, 0, 3, 2, 4).reshape(B, S, H, DV).astype(q.dtype)


def mlstm_mixer(y, w_in, b_gates, head_g, w_out):
    B, S, _ = y.shape
    proj = y @ w_in
    o1 = A_Q_COLS
    o2 = o1 + A_Q_COLS
    o3 = o2 + A_V_COLS
    o4 = o3 + A_V_COLS
    o5 = o4 + A_HEADS
    q = proj[..., :o1].reshape(B, S, A_HEADS, A_QK_DIM)
    k = proj[..., o1:o2].reshape(B, S, A_HEADS, A_QK_DIM) * (A_QK_DIM ** -0.5)
    v = proj[..., o2:o3].reshape(B, S, A_HEADS, A_V_DIM)
    o_pre = proj[..., o3:o4]
    i_pre = (proj[..., o4:o5] + b_gates[0]).astype(jnp.float32)
    log_f = jax.nn.log_sigmoid((proj[..., o5:] + b_gates[1]).astype(jnp.float32))
    h = mlstm_chunkwise(q, k, v, i_pre, log_f)
    h = rms_norm(h, head_g.reshape(A_HEADS, A_V_DIM))
    h = h.reshape(B, S, A_V_COLS) * jax.nn.sigmoid(o_pre)
    return h @ w_out


def shared_kv(h, c_act, kv_norm_g, kv_ada_w, kv_ada_b, kv_w, cos, sin):
    B, S, _ = h.shape
    sh, sc = jnp.split((c_act @ kv_ada_w + kv_ada_b)[:, None, :], 2, axis=-1)
    y = modulate(rms_norm(h, kv_norm_g), sh, sc)
    kv = y @ kv_w
    k = kv[..., :B_K_COLS].reshape(B, S, B_HEADS, 2, B_HEAD_DIM)
    k = apply_partial_rope(k, cos, sin).transpose(0, 2, 3, 1, 4)
    v = kv[..., B_K_COLS:].reshape(B, S, B_HEADS, B_V_DIM).transpose(0, 2, 1, 3)
    return k[:, :, 0], k[:, :, 1], v


def diff_attention(y, k1, k2, v, w_q, lam_params, subln_g, w_out, cos, sin, lam_init):
    B, S, _ = y.shape
    q = (y @ w_q).reshape(B, S, B_HEADS, 2, B_HEAD_DIM)
    q = apply_partial_rope(q, cos, sin) * (B_HEAD_DIM ** -0.5)
    q = q.transpose(0, 2, 3, 1, 4)
    q1, q2 = q[:, :, 0], q[:, :, 1]
    lp = lam_params.astype(jnp.float32)
    lam = jnp.exp(jnp.sum(lp[0] * lp[1])) - jnp.exp(jnp.sum(lp[2] * lp[3])) + lam_init
    outs = []
    for blk in range(S // Q_BLOCK):
        q0 = blk * Q_BLOCK
        kv_len = q0 + Q_BLOCK
        mask = jnp.arange(kv_len)[None, :] <= jnp.arange(q0, kv_len)[:, None]
        s1 = jnp.einsum('bhqd,bhkd->bhqk', q1[:, :, q0:kv_len], k1[:, :, :kv_len]).astype(jnp.float32)
        s2 = jnp.einsum('bhqd,bhkd->bhqk', q2[:, :, q0:kv_len], k2[:, :, :kv_len]).astype(jnp.float32)
        p1 = jax.nn.softmax(jnp.where(mask, s1, -jnp.inf), axis=-1)
        p2 = jax.nn.softmax(jnp.where(mask, s2, -jnp.inf), axis=-1)
        a = (p1 - lam * p2).astype(v.dtype)
        outs.append(jnp.einsum('bhqk,bhkv->bhqv', a, v[:, :, :kv_len]))
    o = jnp.concatenate(outs, axis=2)
    o = rms_norm(o, subln_g, SUBLN_EPS) * (1 - lam_init)
    o = o.transpose(0, 2, 1, 3).reshape(B, S, B_HEADS * B_V_DIM)
    return o @ w_out


def setup_inputs(seed: int = 0) -> dict:
    key = jax.random.key(seed)
    ks = jax.random.split(key, 24)
    nrm = jax.random.normal
    f32 = jnp.float32
    x = nrm(ks[0], (BATCH, SEQ, D_MODEL), f32)
    c = nrm(ks[1], (BATCH, D_MODEL), f32)
    positions = (jnp.arange(SEQ, dtype=jnp.int32)[None, :]
                 + jax.random.randint(ks[2], (BATCH, 1), 0, 1024, dtype=jnp.int32))
    ada_w = nrm(ks[3], (DEPTH, D_MODEL, 6 * D_MODEL), f32) * (0.5 * D_MODEL ** -0.5)
    ada_b = 0.02 * nrm(ks[4], (DEPTH, 6 * D_MODEL), f32)
    norm_mix_g = 1.0 + 0.02 * nrm(ks[5], (DEPTH, D_MODEL), f32)
    norm_mlp_g = 1.0 + 0.02 * nrm(ks[6], (DEPTH, D_MODEL), f32)
    mlp_w_up = nrm(ks[7], (DEPTH, D_MODEL, D_FF), f32) * D_MODEL ** -0.5
    mlp_w_down = nrm(ks[8], (DEPTH, D_FF, D_MODEL), f32) * D_FF ** -0.5
    mlstm_w_in = nrm(ks[9], (N_A_LAYERS, D_MODEL, A_IN_COLS), f32) * D_MODEL ** -0.5
    b_i = 0.1 * nrm(ks[10], (N_A_LAYERS, A_HEADS), f32)
    b_f = jnp.linspace(3.0, 6.0, A_HEADS, dtype=f32)[None, :] + 0.1 * nrm(ks[11], (N_A_LAYERS, A_HEADS), f32)
    mlstm_b_gates = jnp.stack([b_i, b_f], axis=1)
    mlstm_head_g = 1.0 + 0.02 * nrm(ks[12], (N_A_LAYERS, A_V_COLS), f32)
    mlstm_w_out = nrm(ks[13], (N_A_LAYERS, A_V_COLS, D_MODEL), f32) * A_V_COLS ** -0.5
    kv_norm_g = 1.0 + 0.02 * nrm(ks[14], (D_MODEL,), f32)
    kv_ada_w = nrm(ks[15], (D_MODEL, 2 * D_MODEL), f32) * (0.5 * D_MODEL ** -0.5)
    kv_ada_b = 0.02 * nrm(ks[16], (2 * D_MODEL,), f32)
    kv_w = nrm(ks[17], (D_MODEL, B_KV_COLS), f32) * D_MODEL ** -0.5
    diff_w_q = nrm(ks[18], (N_B_LAYERS, D_MODEL, B_Q_COLS), f32) * D_MODEL ** -0.5
    diff_lambda = 0.1 * nrm(ks[19], (N_B_LAYERS, 4, B_HEAD_DIM), f32)
    diff_subln_g = 1.0 + 0.02 * nrm(ks[20], (N_B_LAYERS, B_V_DIM), f32)
    diff_w_out = nrm(ks[21], (N_B_LAYERS, B_HEADS * B_V_DIM, D_MODEL), f32) * (B_HEADS * B_V_DIM) ** -0.5
    final_norm_g = 1.0 + 0.02 * nrm(ks[22], (D_MODEL,), f32)
    return {"x": x, "c": c, "positions": positions,
            "ada_w": ada_w, "ada_b": ada_b,
            "norm_mix_g": norm_mix_g, "norm_mlp_g": norm_mlp_g,
            "mlp_w_up": mlp_w_up, "mlp_w_down": mlp_w_down,
            "mlstm_w_in": mlstm_w_in, "mlstm_b_gates": mlstm_b_gates,
            "mlstm_head_g": mlstm_head_g, "mlstm_w_out": mlstm_w_out,
            "kv_norm_g": kv_norm_g, "kv_ada_w": kv_ada_w, "kv_ada_b": kv_ada_b, "kv_w": kv_w,
            "diff_w_q": diff_w_q, "diff_lambda": diff_lambda,
            "diff_subln_g": diff_subln_g, "diff_w_out": diff_w_out,
            "final_norm_g": final_norm_g}


def reference(x, c, positions, ada_w, ada_b, norm_mix_g, norm_mlp_g, mlp_w_up, mlp_w_down,
              mlstm_w_in, mlstm_b_gates, mlstm_head_g, mlstm_w_out,
              kv_norm_g, kv_ada_w, kv_ada_b, kv_w,
              diff_w_q, diff_lambda, diff_subln_g, diff_w_out, final_norm_g):
    c_act = jax.nn.silu(c)
    cos, sin = rope_tables(positions)
    k1 = k2 = v = None
    for layer in range(DEPTH):
        mod = (c_act @ ada_w[layer] + ada_b[layer])[:, None, :]
        sh1, sc1, g1, sh2, sc2, g2 = jnp.split(mod, 6, axis=-1)
        y = modulate(rms_norm(x, norm_mix_g[layer]), sh1, sc1)
        if layer < N_A_LAYERS:
            mix = mlstm_mixer(y, mlstm_w_in[layer], mlstm_b_gates[layer],
                              mlstm_head_g[layer], mlstm_w_out[layer])
        else:
            j = layer - N_A_LAYERS
            mix = diff_attention(y, k1, k2, v, diff_w_q[j], diff_lambda[j], diff_subln_g[j],
                                 diff_w_out[j], cos, sin, lambda_init(layer + 1))
        x = x + g1 * mix
        y = modulate(rms_norm(x, norm_mlp_g[layer]), sh2, sc2)
        x = x + g2 * sq_relu_mlp(y, mlp_w_up[layer], mlp_w_down[layer])
        if layer == N_A_LAYERS - 1:
            k1, k2, v = shared_kv(x, c_act, kv_norm_g, kv_ada_w, kv_ada_b, kv_w, cos, sin)
    return rms_norm(x, final_norm_g)
```

```python
import numpy as np
import ml_dtypes
from contextlib import ExitStack
import concourse.bass as bass
import concourse.mybir as mybir
from concourse.bass_utils import run_bass_kernel_spmd

F32 = mybir.dt.float32
BF16 = mybir.dt.bfloat16
I32 = mybir.dt.int32
AF = mybir.ActivationFunctionType
ALU = mybir.AluOpType
AX = mybir.AxisListType

D = 1024
S = 2048
DEPTH = 4
DFF = 4096
NCH = 8
TT = 512
NTT = S // TT
NORM_EPS = 1e-6


import os
PARANOID = bool(int(os.environ.get("PARANOID", "0")))


class T:
    __slots__ = ("w", "r", "name")

    def __init__(self, name=""):
        self.w = None
        self.r = {}
        self.name = name


class Prog:
    ENGS = ("pe", "act", "dve", "pool", "sp")

    def __init__(self):
        self.queues = {e: [] for e in self.ENGS}
        self.cnt = {}
        self.known = {e: {} for e in self.ENGS}
        self.dma_rr = {}
        self.NSLOT = {"d_sp": 40, "d_w": 8, "d_out": 8}

    def op(self, eng, meth, kw, reads=(), writes=(), inc=True, dma=None):
        fn = (meth, kw)
        deps = {}

        def add(k, v):
            if eng == "pe" and k == "pe" and dma is None:
                return
            if deps.get(k, 0) < v:
                deps[k] = v

        for t in reads:
            if t.w is not None:
                add(*t.w)
        for t in writes:
            if t.w is not None:
                add(*t.w)
            for k, v in t.r.items():
                add(k, v)
        if PARANOID:
            for k, v in self.cnt.items():
                if v > 0:
                    add(k, v)
        if dma is not None:
            n = self.dma_rr.get(dma, 0)
            self.dma_rr[dma] = n + 1
            key, step = f"{dma}_{n % self.NSLOT.get(dma, 8)}", 16
            if self.cnt.get(key, 0) > 0:
                add(key, self.cnt[key])
        else:
            key, step = eng, 1
        kn = self.known[eng]
        waits = []
        for k, v in deps.items():
            if kn.get(k, 0) < v:
                kn[k] = v
                waits.append((k, v))
        if key not in self.cnt:
            self.cnt[key] = 0
        if inc:
            self.cnt[key] += step
            tok = (key, self.cnt[key])
        else:
            tok = (key, self.cnt[key] + step)
        self.queues[eng].append((waits, fn, key if inc else None, step))
        for t in reads:
            if t.r.get(tok[0], 0) < tok[1]:
                t.r[tok[0]] = tok[1]
        for t in writes:
            t.w = tok
            t.r = {}
        return tok

    def wait_all(self, eng):
        kn = self.known[eng]
        waits = []
        for k, v in self.cnt.items():
            if v > 0 and kn.get(k, 0) < v:
                kn[k] = v
                waits.append((k, v))
        self.queues[eng].append((waits, None, None, 0))

    def barrier(self):
        for e in self.ENGS:
            self.wait_all(e)

    def emit(self, nc, block, sems):
        decos = {"pe": block.tensor, "act": block.scalar, "dve": block.vector,
                 "pool": block.gpsimd, "sp": block.sync}
        for e in self.ENGS:
            q = self.queues[e]

            def body(engine, q=q):
                for waits, fn, key, step in q:
                    for k, v in waits:
                        engine.wait_ge(sems[k], v)
                    if fn is None:
                        continue
                    ins = getattr(engine, fn[0])(**fn[1])
                    if key is not None:
                        ins.then_inc(sems[key], step)

            decos[e](body)


def fm(v):
    v = np.asarray(v, dtype=np.float32)
    return np.ascontiguousarray(v.reshape(-1, 128).T)


ROPE_THETA = 500000.0
LAM_INIT = [0.8 - 0.6 * float(np.exp(-0.3 * (n - 1))) for n in (3, 4)]


def make_consts():
    ident = np.eye(128, dtype=np.float32)
    tri = np.triu(np.ones((128, 128), dtype=np.float32))
    pm = np.zeros((128, 128), dtype=np.float32)
    invf = np.zeros((128, 1), dtype=np.float32)
    inv_freq = ROPE_THETA ** (-np.arange(0, 16, 2, dtype=np.float32) / 16.0)
    for m in range(128):
        d = m % 64
        if d < 8:
            pm[m + 8, m] = -1.0
            invf[m, 0] = inv_freq[d]
        elif d < 16:
            pm[m - 8, m] = 1.0
            invf[m, 0] = inv_freq[d - 8]
    return np.ascontiguousarray(np.concatenate([ident, tri, pm, invf], axis=1))


def pack_vecs(inp):
    cols = {}
    parts = []
    off = 0

    def put(name, arr):
        nonlocal off
        cols[name] = (off, arr.shape[1])
        parts.append(arr)
        off += arr.shape[1]

    for l in range(DEPTH):
        put(f"ada_b{l}", fm(inp["ada_b"][l]))
    put("kv_ada_b", fm(inp["kv_ada_b"]))
    for l in range(DEPTH):
        put(f"nmix{l}", fm(inp["norm_mix_g"][l]))
        put(f"nmlp{l}", fm(inp["norm_mlp_g"][l]))
    put("kvn", fm(inp["kv_norm_g"]))
    put("fin", fm(inp["final_norm_g"]))
    bg = np.zeros((128, 4), dtype=np.float32)
    for l in range(2):
        for w in range(2):
            bg[0:4, l * 2 + w] = np.asarray(inp["mlstm_b_gates"], dtype=np.float32)[l, w]
    put("bg", bg)
    for l in range(2):
        put(f"hg{l}", fm(inp["mlstm_head_g"][l]))
    return np.ascontiguousarray(np.concatenate(parts, axis=1)), cols


def build(nseq, vec_cols, nvec, mode="full", debug=False):
    do_mlstm = mode in ("full", "mlstm")
    do_attn = mode in ("full", "attn")
    nc = bass.Bass("TRN2", target_bir_lowering=False)
    P = Prog()

    def dram(name, shape, dt=F32, kind="ExternalInput"):
        return nc.dram_tensor(name, list(shape), dt, kind=kind).ap()

    x_t = dram("x_t", [nseq, D, S])
    c_t = dram("c_t", [128, NCH, nseq])
    pos_d = dram("positions", [nseq, S], I32)
    vecs_d = dram("vecs", [128, nvec])
    consts_d = dram("consts", [128, 385])
    ada_w = dram("ada_w", [DEPTH, D, 6 * D])
    kv_ada_w = dram("kv_ada_w", [D, 2 * D])
    w_up = dram("mlp_w_up", [DEPTH, D, DFF])
    w_dn = dram("mlp_w_down", [DEPTH, DFF, D])
    ml_w_in = dram("mlstm_w_in", [2, D, 3080])
    ml_w_out = dram("mlstm_w_out", [2, D, D])
    ml_head_g = dram("mlstm_head_g", [2, D])
    kv_w = dram("kv_w", [D, 2 * D])
    df_w_q = dram("diff_w_q", [2, D, D])
    df_w_out = dram("diff_w_out", [2, D, D])
    df_lam = dram("diff_lambda", [2, 256])
    df_subg = dram("diff_subln_g", [2, 128])
    out_t = dram("out_t", [nseq, D, S], kind="ExternalOutput")
    SK = "ExternalOutput" if debug else "Internal"
    qT_s = dram("qT_s", [8, 128, S], BF16, kind=SK)
    kT_s = dram("kT_s", [8, 128, S], BF16, kind=SK)
    v_s = dram("v_s", [16, 128, D], BF16, kind=SK)
    ktm_s = dram("ktm_s", [16, 128, 512], BF16, kind="Internal")
    osig_s = dram("osig_s", [16, 128, D], BF16, kind="Internal")
    cos_s = dram("cos_s", [128, S], F32, kind=SK)
    sin_s = dram("sin_s", [128, S], F32, kind=SK)
    gi_s = dram("gi_s", [4, S], F32, kind="Internal")
    gf_s = dram("gf_s", [4, S], F32, kind="Internal")
    sc1_s = dram("sc1_s", [64, 2], F32, kind="Internal")
    sc2_s = dram("sc2_s", [4, 16], F32, kind="Internal")
    negu_s = dram("negu_s", [64, 128], F32, kind="Internal")
    wint_s = dram("wint_s", [64, 128], F32, kind="Internal")
    wold_s = dram("wold_s", [64, 1], F32, kind="Internal")
    scr_t = {n: T(n) for n in ("qT", "kT", "v", "ktm", "osig", "cs", "gi", "gf", "sc1", "sc2", "negu", "wint", "wold")}

    dbg_list = []

    def dbg(name, src_ap, shape, dt, reads):
        if not debug:
            return
        d = nc.dram_tensor("dbg_" + name, list(shape), dt, kind="ExternalOutput").ap()
        P.op("sp", "dma_start", dict(out=d, in_=src_ap), reads=reads, dma="d_out")
        dbg_list.append("dbg_" + name)

    es = ExitStack()
    with es:
        def sb(name, shape, dt=F32):
            return es.enter_context(nc.sbuf_tensor(name, list(shape), dt))

        def ps(name, shape, dt=F32):
            return es.enter_context(nc.psum_tensor(name, list(shape), dt))

        xT = sb("xT", [128, NCH, S])
        xT_t = [[T() for t in range(NTT)] for k in range(NCH)]
        yT = sb("yT", [128, NCH, S], BF16)
        yT_t = [[T() for t in range(NTT)] for k in range(NCH)]
        vecs = sb("vecs_sb", [128, nvec]); vecs_t = T()
        consts = sb("consts_sb", [128, 385]); consts_t = T()
        identf = consts[:, 0:128]; trif = consts[:, 128:256]; pmf = consts[:, 256:384]; invf = consts[:, 384:385]
        cbf = sb("consts_bf", [128, 384], BF16)
        ones_bf = cbf[:, 0:128]; ident_bf = cbf[:, 128:256]
        cT = sb("cT", [128, NCH, nseq]); cT_bf = sb("cT_bf", [128, NCH, nseq], BF16); cT_t = T()
        modT = [sb(f"mod{l}", [128, 48, nseq]) for l in range(DEPTH)]
        modkv = sb("modkv", [128, 16, nseq])
        mod_t = [T() for l in range(DEPTH + 1)]
        gsc = [[sb(f"gsc{l}_{w}", [128, NCH, nseq]) for w in range(2)] for l in range(DEPTH)]
        gsckv = sb("gsckv", [128, NCH, nseq]); gsc_t = T()
        NW = 4
        wb = [sb(f"wb{i}", [128, NCH, 512], BF16) for i in range(NW)]
        wb_t = [T() for i in range(NW)]
        hbuf = [sb(f"hbuf{i}", [128, 4, TT], BF16) for i in range(2)]
        hbuf_t = [[T() for c in range(4)] for i in range(2)]
        sq = sb("sq", [128, 4, TT], BF16); sq_t = T()
        NTMP = 6
        tmp = [sb(f"tmp{i}", [128, TT]) for i in range(NTMP)]
        tmp_t = [T() for i in range(NTMP)]
        rstd = [sb(f"rstd{i}", [128, TT]) for i in range(2)]; rstd_t = [T() for i in range(2)]
        stg = [sb(f"stg{i}", [128, 1024], BF16) for i in range(2)]
        stg_t = [T() for i in range(2)]
        small = sb("small", [128, 512]); small_t = T()
        mxa = sb("mxa", [128, 2304], BF16)
        mxa_t = [T(), T()]
        mxv = sb("mxv", [128, 16 * 129 + 8], BF16)
        mxv_t = [T(), T()]
        mxq = [sb(f"mxq{i}", [128, 512], BF16) for i in range(2)]; mxq_t = [T(), T()]
        ebuf = [sb(f"ebuf{i}", [128, 512], BF16) for i in range(4)]; ebuf_t = [T() for i in range(4)]
        obuf = sb("obuf", [128, 1032]); obuf_t = T()
        cst = sb("cst", [128, 4, 258]); cst_t = [T() for h in range(4)]
        cstb = sb("cstb", [128, 4, 257], BF16); cstb_t = [T() for h in range(4)]
        lamv = sb("lamv", [128, 8]); lam_t = T()
        gsub = [sb(f"gsub{j}", [128, 128]) for j in range(2)]; gsub_t = T()
        cols3 = sb("cols3", [128, 4, 64]); cols3_t = T()
        cnt = {"ps": 0, "tmp": 0, "stg": 0, "h": 0, "rstd": 0, "w": 0, "e": 0, "mxq": 0}

        pall = ps("pall", [128, 4096])
        psb = [pall[:, i * 512:(i + 1) * 512] for i in range(8)]
        psb_t = [T() for i in range(8)]
        psacc = pall[:, 2048:4096]

        def next_ps():
            i = cnt["ps"] % 8
            cnt["ps"] += 1
            return psb[i], psb_t[i]

        def next_tmp():
            i = cnt["tmp"] % NTMP
            cnt["tmp"] += 1
            return tmp[i], tmp_t[i]

        def next_stg():
            i = cnt["stg"] % 2
            cnt["stg"] += 1
            return stg[i], stg_t[i]

        def MM(out, lhsT, rhs, start, stop, reads, writes, inc):
            P.op("pe", "matmul", dict(out=out, lhsT=lhsT, rhs=rhs, start=start, stop=stop), reads, writes, inc=inc)

        def ACT(out, in_, func, reads, writes, bias=None, scale=None):
            kw = dict(out=out, in_=in_, func=func)
            if bias is not None:
                kw["bias"] = bias
            if scale is not None:
                kw["scale"] = scale
            P.op("act", "activation", kw, reads, writes)

        def TTo(eng, out, in0, in1, op, reads, writes):
            P.op(eng, "tensor_tensor", dict(out=out, in0=in0, in1=in1, op=op), reads, writes)

        def STT(out, in0, scalar, in1, op0, op1, reads, writes):
            P.op("dve", "scalar_tensor_tensor", dict(out=out, in0=in0, scalar=scalar, in1=in1, op0=op0, op1=op1),
                 reads, writes)

        def TS(eng, out, in0, s1, s2, op0, op1, reads, writes):
            P.op(eng, "tensor_scalar", dict(out=out, in0=in0, scalar1=s1, scalar2=s2, op0=op0, op1=op1), reads, writes)

        def CP(eng, out, in_, reads, writes):
            if eng == "act":
                P.op("act", "activation", dict(out=out, in_=in_, func=AF.Copy), reads, writes)
            else:
                P.op(eng, "tensor_copy", dict(out=out, in_=in_), reads, writes)

        cur = {"b": 0}

        def DMA(eng, out, in_, reads, writes, sem="d_sp"):
            return P.op(eng, "dma_start", dict(out=out, in_=in_), reads, writes, dma=sem)

        def load_w(src_ap, view=None):
            i = cnt["w"] % NW
            cnt["w"] += 1
            dst = wb[i][:] if view is None else view(wb[i])
            DMA("pool", dst, src_ap, [], [wb_t[i]], sem="d_w")
            return wb[i], wb_t[i]

        DMA("sp", vecs[:], vecs_d[:, :], [], [vecs_t])
        DMA("sp", consts[:], consts_d[:, :], [], [consts_t])
        DMA("sp", cT[:], c_t[:, :, :], [], [cT_t])
        P.op("pool", "memset", dict(ap=cbf[:, 0:128], constant=1.0), [], [consts_t])
        CP("pool", ident_bf, identf, [consts_t], [consts_t])
        P.op("pool", "memset", dict(ap=mxv[:], constant=1.0), [], mxv_t)
        ACT(cT_bf[:], cT[:], AF.Silu, [cT_t], [cT_t])

        jobs = [(l, pc) for l in range(DEPTH) for pc in range(12)] + [(DEPTH, pc) for pc in range(4)]

        def ada_src(l, pc):
            src = ada_w[l] if l < DEPTH else kv_ada_w
            return src.rearrange("(k p) n -> p k n", p=128)[:, :, pc * 512:(pc + 1) * 512]

        pend = [load_w(ada_src(*jobs[0]))]
        for i, (l, pc) in enumerate(jobs):
            if i + 1 < len(jobs):
                pend.append(load_w(ada_src(*jobs[i + 1])))
            w, wt = pend.pop(0)
            pt, ptt = next_ps()
            for jj in range(4):
                for k in range(NCH):
                    MM(pt[:, jj * nseq:(jj + 1) * nseq], w[:, k, jj * 128:(jj + 1) * 128], cT_bf[:, k, :],
                       k == 0, k == NCH - 1, [wt, cT_t], [ptt], inc=(jj == 3 and k == NCH - 1))
            dst = modT[l] if l < DEPTH else modkv
            bo = vec_cols[f"ada_b{l}" if l < DEPTH else "kv_ada_b"][0] + pc * 4
            bias_ap = bass.AP(vecs, bo, [[nvec, 128], [1, 4], [0, nseq]])
            TTo("dve", dst[:, pc * 4:(pc + 1) * 4, :], pt[:, 0:4 * nseq].rearrange("p (j b) -> p j b", b=nseq),
                bias_ap, ALU.add, [ptt, vecs_t], [mod_t[l]])
        for l in range(DEPTH):
            for w_ in range(2):
                go = vec_cols[f"nmix{l}" if w_ == 0 else f"nmlp{l}"][0]
                g_ap = bass.AP(vecs, go, [[nvec, 128], [1, NCH], [0, nseq]])
                sc0 = (1 + 3 * w_) * NCH
                STT(gsc[l][w_][:], modT[l][:, sc0:sc0 + NCH, :], 1.0, g_ap, ALU.add, ALU.mult,
                    [mod_t[l], vecs_t], [gsc_t])
        g_ap = bass.AP(vecs, vec_cols["kvn"][0], [[nvec, 128], [1, NCH], [0, nseq]])
        STT(gsckv[:], modkv[:, NCH:2 * NCH, :], 1.0, g_ap, ALU.add, ALU.mult, [mod_t[DEPTH], vecs_t], [gsc_t])

        if do_attn:
            for j in range(2):
                t0, t0t = next_tmp()
                DMA("sp", t0[:, 0:256], bass.AP(df_lam.tensor, j * 256, [[0, 128], [1, 256]]), [], [t0t])
                t1, t1t = next_tmp()
                TTo("dve", t1[:, 0:64], t0[:, 0:64], t0[:, 64:128], ALU.mult, [t0t], [t1t])
                TTo("dve", t1[:, 64:128], t0[:, 128:192], t0[:, 192:256], ALU.mult, [t0t], [t1t])
                P.op("dve", "tensor_reduce", dict(out=lamv[:, 4 + 2 * j:6 + 2 * j],
                                                  in_=t1[:, 0:128].rearrange("p (a b) -> p a b", a=2),
                                                  axis=AX.X, op=ALU.add), [t1t], [lam_t])
                ACT(lamv[:, 4 + 2 * j:6 + 2 * j], lamv[:, 4 + 2 * j:6 + 2 * j], AF.Exp, [lam_t], [lam_t])
                TTo("dve", lamv[:, 2 * j:2 * j + 1], lamv[:, 4 + 2 * j:5 + 2 * j], lamv[:, 5 + 2 * j:6 + 2 * j],
                    ALU.subtract, [lam_t], [lam_t])
                TS("dve", lamv[:, 2 * j:2 * j + 1], lamv[:, 2 * j:2 * j + 1], LAM_INIT[j], None, ALU.add, ALU.bypass,
                   [lam_t], [lam_t])
                TS("dve", lamv[:, 2 * j + 1:2 * j + 2], lamv[:, 2 * j:2 * j + 1], -1.0, None, ALU.mult, ALU.bypass,
                   [lam_t], [lam_t])
                DMA("sp", gsub[j][:], bass.AP(df_subg.tensor, j * 128, [[0, 128], [1, 128]]), [], [gsub_t])
                TS("dve", gsub[j][:], gsub[j][:], 1.0 - LAM_INIT[j], None, ALU.mult, ALU.bypass, [gsub_t], [gsub_t])

        def norm_tile(tt, out_fn, eps=NORM_EPS):
            tsl = slice(tt * TT, (tt + 1) * TT)
            pt, ptt = next_ps()
            for hf in range(2):
                TTo("pool", sq[:], xT[:, hf * 4:hf * 4 + 4, tsl], xT[:, hf * 4:hf * 4 + 4, tsl], ALU.mult,
                    [xT_t[k][tt] for k in range(hf * 4, hf * 4 + 4)], [sq_t])
                for k4 in range(4):
                    k = hf * 4 + k4
                    MM(pt[:], ones_bf, sq[:, k4, :], k == 0, k == NCH - 1, [consts_t, sq_t], [ptt], inc=(k4 == 3))
            ri = cnt["rstd"] % 2
            cnt["rstd"] += 1
            lt, ltt = next_tmp()
            ACT(lt[:], pt[:], AF.Ln, [ptt], [ltt], bias=eps, scale=1.0 / D)
            ACT(rstd[ri][:], lt[:], AF.Exp, [ltt], [rstd_t[ri]], scale=-0.5)
            for k in range(NCH):
                xr, xrt = next_tmp()
                TTo("dve", xr[:], xT[:, k, tsl], rstd[ri][:], ALU.mult, [xT_t[k][tt], rstd_t[ri]], [xrt])
                out_fn(k, xr, xrt)

        def norm_to_y(b, gain, shift_mod, shift_off):
            for tt in range(NTT):
                tsl = slice(tt * TT, (tt + 1) * TT)

                def out_fn(k, xr, xrt, tt=tt, tsl=tsl):
                    ACT(yT[:, k, tsl], xr[:], AF.Identity, [xrt, gsc_t] + mod_t, [yT_t[k][tt]],
                        bias=shift_mod[:, shift_off + k, b:b + 1], scale=gain[:, k, b:b + 1])

                norm_tile(tt, out_fn)

        def x_update(pt, ptt, n, tt, gate_ap):
            tsl = slice(tt * TT, (tt + 1) * TT)
            STT(xT[:, n, tsl], pt[:], gate_ap, xT[:, n, tsl], ALU.mult, ALU.add,
                [ptt, xT_t[n][tt]] + mod_t, [xT_t[n][tt]])

        prefetched = {}

        def dview(w):
            return w[:].rearrange("p k n -> p (k n)").rearrange("p (k n) -> p k n", k=4)

        def mlp_load(l, j):
            su = w_up[l].rearrange("(k p) n -> p k n", p=128)[:, :, j * 512:(j + 1) * 512]
            sd = w_dn[l].rearrange("(k p) n -> p k n", p=128)[:, j * 4:(j + 1) * 4, :]
            return load_w(su), load_w(sd, view=dview)

        def mlp_prefetch(b, l):
            prefetched[("mlp", b, l)] = mlp_load(l, 0)

        def mlp(b, l):
            g2 = modT[l]

            def load(j):
                return mlp_load(l, j)

            pend = [prefetched.pop(("mlp", b, l)) if ("mlp", b, l) in prefetched else load(0)]
            for j in range(8):
                if j + 1 < 8:
                    pend.append(load(j + 1))
                (wu, wut), (wd_, wdt) = pend.pop(0)
                wd = dview(wd_)
                for tt in range(NTT):
                    tsl = slice(tt * TT, (tt + 1) * TT)
                    hi = cnt["h"] % 2
                    cnt["h"] += 1
                    for hc in range(4):
                        pt, ptt = next_ps()
                        for k in range(NCH):
                            MM(pt[:], wu[:, k, hc * 128:(hc + 1) * 128], yT[:, k, tsl], k == 0, k == NCH - 1,
                               [wut, yT_t[k][tt]], [ptt], inc=(k == NCH - 1))
                        rl, rlt = next_tmp()
                        ACT(rl[:], pt[:], AF.Relu, [ptt], [rlt])
                        TTo("dve", hbuf[hi][:, hc, :], rl[:], rl[:], ALU.mult, [rlt], [hbuf_t[hi][hc]])
                    for n in range(NCH):
                        pt, ptt = next_ps()
                        for kc in range(4):
                            MM(pt[:], wd[:, kc, n * 128:(n + 1) * 128], hbuf[hi][:, kc, :], kc == 0, kc == 3,
                               [wdt, hbuf_t[hi][kc]], [ptt], inc=(kc == 3))
                        x_update(pt, ptt, n, tt, g2[:, 5 * NCH + n, b:b + 1])

        def out_proj(b, l, wsrc):
            g1 = modT[l]
            if ("op", b, l) in prefetched:
                pend = prefetched.pop(("op", b, l))
            else:
                pend = [load_w(wsrc.rearrange("(k p) n -> p k n", p=128)[:, :, 0:512])]
            for pc in range(2):
                if pc == 0 and len(pend) < 2:
                    pend.append(load_w(wsrc.rearrange("(k p) n -> p k n", p=128)[:, :, 512:1024]))
                w, wt = pend.pop(0)
                for tt in range(NTT):
                    tsl = slice(tt * TT, (tt + 1) * TT)
                    for n4 in range(4):
                        pt, ptt = next_ps()
                        for k in range(NCH):
                            MM(pt[:], w[:, k, n4 * 128:(n4 + 1) * 128], yT[:, k, tsl], k == 0, k == NCH - 1,
                               [wt, yT_t[k][tt]], [ptt], inc=(k == NCH - 1))
                        x_update(pt, ptt, pc * 4 + n4, tt, g1[:, 2 * NCH + pc * 4 + n4, b:b + 1])

        def rope_tables(b):
            for tt in range(NTT):
                tsl = slice(tt * TT, (tt + 1) * TT)
                pi_, pit = next_tmp()
                pi_i = pi_[:].bitcast(I32)
                DMA("sp", pi_i, bass.AP(pos_d.tensor, b * S + tt * TT, [[0, 128], [1, TT]]), [], [pit])
                pf, pft = next_tmp()
                CP("dve", pf[:], pi_i, [pit], [pft])
                ang, angt = next_tmp()
                TS("dve", ang[:], pf[:], invf, 1.0 / (2 * np.pi), ALU.mult, ALU.mult, [pft, consts_t], [angt])
                for which, dst in ((0, sin_s), (1, cos_s)):
                    t, ttk = next_tmp()
                    if which == 1:
                        TS("dve", t[:], ang[:], 0.25, None, ALU.add, ALU.bypass, [angt], [ttk])
                        src = t
                    else:
                        src = ang
                    ki, kit = next_tmp()
                    ki_i = ki[:].bitcast(I32)
                    CP("dve", ki_i, src[:], [angt, ttk], [kit])
                    kf, kft = next_tmp()
                    CP("dve", kf[:], ki_i, [kit], [kft])
                    r, rt = next_tmp()
                    TTo("dve", r[:], src[:], kf[:], ALU.subtract, [angt, ttk, kft], [rt])
                    m1, m1t = next_tmp()
                    TS("dve", m1[:], r[:], 0.5, None, ALU.is_gt, ALU.bypass, [rt], [m1t])
                    TTo("dve", r[:], r[:], m1[:], ALU.subtract, [rt, m1t], [rt])
                    TS("dve", m1[:], r[:], -0.5, None, ALU.is_lt, ALU.bypass, [rt, m1t], [m1t])
                    TTo("dve", r[:], r[:], m1[:], ALU.add, [rt, m1t], [rt])
                    ACT(r[:], r[:], AF.Sin, [rt], [rt], scale=2 * np.pi * (1 - 1e-6))
                    DMA("sp", dst[:, tsl], r[:], [rt], [scr_t["cs"]])

        def rope_proj(b, wsrc, dst_s, dst_t):
            wv = wsrc.rearrange("(k p) n -> p k n", p=128)
            pend = [load_w(wv[:, :, 0:512]), load_w(wv[:, :, 512:1024])]
            for tt in range(NTT):
                tsl = slice(tt * TT, (tt + 1) * TT)
                cs = obuf[:, 0:512]; sn = obuf[:, 512:1024]; cst_ = obuf_t; snt = obuf_t
                DMA("sp", cs, cos_s[:, tsl], [scr_t["cs"]], [cst_])
                DMA("sp", sn, sin_s[:, tsl], [scr_t["cs"]], [snt])
                for pc in range(2):
                    w, wt = pend[pc]
                    for hk in range(4):
                        h = pc * 4 + hk
                        pt, ptt = next_ps()
                        for k in range(NCH):
                            MM(pt[:], w[:, k, hk * 128:(hk + 1) * 128], yT[:, k, tsl], k == 0, k == NCH - 1,
                               [wt, yT_t[k][tt]], [ptt], inc=(k == NCH - 1))
                        kf, kft = next_tmp()
                        ACT(kf[:], pt[:], AF.Copy, [ptt], [kft])
                        pp, ppt = next_ps()
                        MM(pp[:], pmf, kf[:], True, True, [consts_t, kft], [ppt], inc=True)
                        t2, t2t = next_tmp()
                        TTo("dve", t2[:], pp[:], sn, ALU.mult, [ppt, snt], [t2t])
                        TTo("pool", kf[:], kf[:], cs, ALU.mult, [kft, cst_], [kft])
                        st, stt = next_stg()
                        TTo("pool", st[:, 0:512], kf[:], t2[:], ALU.add, [kft, t2t], [stt])
                        DMA("sp", dst_s[h][:, tsl], st[:, 0:512], [stt], [dst_t])

        def kv_stage(b):
            norm_to_y(b, gsckv, modkv, 0)
            rope_proj(b, kv_w[:, 0:1024], kT_s, scr_t["kT"])
            wv = kv_w.rearrange("(k p) n -> p k n", p=128)
            pend = [load_w(wv[:, :, 1024:1536]), load_w(wv[:, :, 1536:2048])]
            for pc in range(2):
                w, wt = pend[pc]
                for blk in range(16):
                    tt = blk // 4
                    bsl = slice(blk * 128, (blk + 1) * 128)
                    pt, ptt = next_ps()
                    for k in range(NCH):
                        MM(pt[:], yT[:, k, bsl], w[:, k, :], k == 0, k == NCH - 1, [wt, yT_t[k][tt]], [ptt],
                           inc=(k == NCH - 1))
                    st, stt = next_stg()
                    ACT(st[:, 0:512], pt[:], AF.Copy, [ptt], [stt])
                    DMA("sp", v_s[blk][:, pc * 512:(pc + 1) * 512], st[:, 0:512], [stt], [scr_t["v"]])

        def attn_mixer(b, l):
            j = l - 2
            norm_to_y(b, gsc[l][0], modT[l], 0)
            rope_proj(b, df_w_q[j], qT_s, scr_t["qT"])
            if b == 0 and j == 0:
                dbg("lamv", lamv[:], [128, 8], F32, [lam_t])
                dbg("x2in", xT[:], [128, NCH, S], F32, [xT_t[k][t] for k in range(NCH) for t in range(NTT)])
            nlam_c = lamv[:, 2 * j + 1:2 * j + 2]
            kbufs = [mxa[:, 0:2048], nw4[:].rearrange("p a h t -> p (a h t)").bitcast(BF16)]
            kts = [[mxa_t[0], mxa_t[1]], [nw4_t]]
            vbufs = [mxv[:, 0:16 * 129].rearrange("p (k v) -> p k v", k=16),
                     cst[:].rearrange("p h v -> p (h v)").bitcast(BF16).rearrange("p (k v) -> p k v", k=16)]
            vts = [[mxv_t[0], mxv_t[1]], list(cst_t)]
            for i_ in range(2):
                P.op("pool", "memset", dict(ap=vbufs[i_][:, :, 128:129], constant=1.0), [], vts[i_])
            accs = obuf[:, 0:1032].rearrange("p (a v) -> p a v", a=8)

            def acc(c, qs):
                o_ = (c * 4 + qs) * 256
                return psacc[:, o_:o_ + 129], psb_t[4 + (c * 4 + qs) // 2]

            prefetched[("op", b, l)] = [load_w(df_w_out[j].rearrange("(k p) n -> p k n", p=128)[:, :, 0:512]),
                                        load_w(df_w_out[j].rearrange("(k p) n -> p k n", p=128)[:, :, 512:1024])]
            for h in range(8):
                hp = h % 2
                kt_sb, ktt = kbufs[hp], kts[hp]
                Vt, vtt = vbufs[hp], vts[hp]
                DMA("sp", kt_sb, kT_s[h][:, :], [scr_t["kT"]], ktt)
                DMA("sp", Vt[:, :, 0:128], bass.AP(v_s.tensor, h * 128, [[D, 128], [128 * D, 16], [1, 128]]),
                    [scr_t["v"]], vtt)
                for tt in range(NTT):
                    tsl = slice(tt * TT, (tt + 1) * TT)
                    nkb = 4 * (tt + 1)
                    qi = cnt["mxq"] % 2
                    cnt["mxq"] += 1
                    q_t = mxq[qi]
                    DMA("sp", q_t[:], qT_s[h][:, tsl], [scr_t["qT"]], [mxq_t[qi]])
                    steps = [(c, kb) for c in range(2) for kb in range(nkb)]
                    info = {}

                    def emit_S(i):
                        c, kb = steps[i]
                        psl = slice(c * 64, (c + 1) * 64)
                        q0 = max(0, kb - 4 * tt)
                        qlo = q0 * 128
                        nq = 512 - qlo
                        si = cnt["ps"] % 3
                        cnt["ps"] += 1
                        spt, sptt = psb[si], psb_t[si]
                        MM(spt[:, 0:nq], kt_sb[psl, kb * 128:(kb + 1) * 128], q_t[psl, qlo:512], True, True,
                           ktt + [mxq_t[qi]], [sptt], inc=True)
                        ei = cnt["e"] % 4
                        cnt["e"] += 1
                        ACT(ebuf[ei][:, 0:nq], spt[:, 0:nq], AF.Exp, [sptt], [ebuf_t[ei]], scale=0.125)
                        if kb >= 4 * tt:
                            TTo("pool", ebuf[ei][:, 0:128], ebuf[ei][:, 0:128], tri_bf, ALU.mult,
                                [ebuf_t[ei], consts_t], [ebuf_t[ei]])
                        info[i] = (ei, q0, qlo)

                    def emit_PV(i):
                        c, kb = steps[i]
                        ei, q0, qlo = info[i]
                        for qs in range(q0, 4):
                            a, at = acc(c, qs)
                            last = (kb == 4 * tt + qs)
                            P.op("pe", "matmul", dict(out=a, lhsT=ebuf[ei][:, qs * 128 - qlo:(qs + 1) * 128 - qlo],
                                                      rhs=Vt[:, kb, :], start=(kb == 0 and qs % 2 == 0), stop=last,
                                                      skip_group_check=True),
                                 [ebuf_t[ei]] + vtt, [at], inc=last)

                    LOOK = 2
                    for i in range(min(LOOK, len(steps))):
                        emit_S(i)
                    for i in range(len(steps)):
                        if i + LOOK < len(steps):
                            emit_S(i + LOOK)
                        emit_PV(i)
                    for c in range(2):
                        CP("act", accs[:, c * 4:(c + 1) * 4, :],
                           psacc[:, c * 1024:(c + 1) * 1024].rearrange("p (q v) -> p q v", q=4)[:, :, 0:129],
                           [psb_t[4 + 2 * c], psb_t[5 + 2 * c]], [obuf_t])
                    rs = small[:, 0:8]
                    P.op("dve", "reciprocal", dict(out=rs, in_=accs[:, :, 128]), [obuf_t], [small_t])
                    TS("dve", small[:, 4:8], small[:, 4:8], nlam_c, None, ALU.mult, ALU.bypass, [small_t, lam_t], [small_t])
                    r1b = bass.AP(small, 0, [[512, 128], [1, 4], [0, 128]])
                    r2b = bass.AP(small, 4, [[512, 128], [1, 4], [0, 128]])
                    tA, tAt = next_tmp()
                    tB, tBt = next_tmp()
                    A3 = tA[:].rearrange("p (q v) -> p q v", q=4)
                    B3 = tB[:].rearrange("p (q v) -> p q v", q=4)
                    TTo("dve", A3, accs[:, 4:8, 0:128], r2b, ALU.mult, [obuf_t, small_t], [tAt])
                    TTo("pool", B3, accs[:, 0:4, 0:128], r1b, ALU.mult, [obuf_t, small_t], [tBt])
                    TTo("dve", B3, B3, A3, ALU.add, [tAt, tBt], [tBt])
                    TTo("pool", A3, B3, B3, ALU.mult, [tBt, tAt], [tAt])
                    P.op("dve", "tensor_reduce", dict(out=small[:, 8:12], in_=A3, axis=AX.X, op=ALU.add), [tAt], [small_t])
                    ACT(small[:, 12:16], small[:, 8:12], AF.Ln, [small_t], [small_t], bias=1e-5, scale=1.0 / 128)
                    ACT(small[:, 16:20], small[:, 12:16], AF.Exp, [small_t], [small_t], scale=-0.5)
                    rsb = bass.AP(small, 16, [[512, 128], [1, 4], [0, 128]])
                    TTo("dve", B3, B3, rsb, ALU.mult, [tBt, small_t], [tBt])
                    st, stt = next_stg()
                    gb = bass.AP(gsub[j], 0, [[128, 128], [0, 4], [1, 128]])
                    TTo("dve", st[:, 0:512].rearrange("p (q v) -> p q v", q=4), B3, gb, ALU.mult, [tBt, gsub_t], [stt])
                    tp = psb[3].bitcast(BF16)
                    for qs in range(4):
                        P.op("pe", "transpose", dict(out=tp[:, qs * 128:(qs + 1) * 128], in_=st[:, qs * 128:(qs + 1) * 128],
                                                     identity=ident_bf), [stt, consts_t], [psb_t[3]], inc=(qs == 3))
                    CP("act", yT[:, h, tsl], tp[:, 0:512], [psb_t[3]], [yT_t[h][tt]])
            if b == 0 and j == 0:
                dbg("onT", yT[:], [128, NCH, S], BF16, [yT_t[k][t] for k in range(NCH) for t in range(NTT)])
            out_proj(b, l, df_w_out[j])
            if b == 0 and j == 0:
                dbg("x2mix", xT[:], [128, NCH, S], F32, [xT_t[k][t] for k in range(NCH) for t in range(NTT)])

        cmp = sb("cmp", [64, 13, 128]); cmp_t = T()
        nw4 = sb("nw4", [128, 2, 4, 128]); nw4_t = T()
        if do_mlstm:
            P.op("pool", "memset", dict(ap=cmp[:, 9, :], constant=1.0), [], [cmp_t])
            P.op("pool", "memset", dict(ap=cmp[:, 10, :], constant=0.0), [], [cmp_t])

        def mlstm_mixer(b, l):
            norm_to_y(b, gsc[l][0], modT[l], 0)
            wv = ml_w_in[l].rearrange("(k p) n -> p k n", p=128)
            SC = 128 ** -0.5
            gi_ = cnt["w"] % NW
            cnt["w"] += 1
            wg, wgt = wb[gi_], wb_t[gi_]
            DMA("pool", wg[:, :, 0:8], wv[:, :, 3072:3080], [], [wgt], sem="d_w")
            bgo = vec_cols["bg"][0]
            for tt in range(NTT):
                tsl = slice(tt * TT, (tt + 1) * TT)
                for which, dst, dk in ((0, gi_s, "gi"), (1, gf_s, "gf")):
                    pt, ptt = next_ps()
                    for k in range(NCH):
                        MM(pt[0:4, :], wg[:, k, which * 4:(which + 1) * 4], yT[:, k, tsl], k == 0, k == NCH - 1,
                           [wgt, yT_t[k][tt]], [ptt], inc=(k == NCH - 1))
                    t, ttk = next_tmp()
                    ACT(t[0:4, :], pt[0:4, :], AF.Identity, [ptt, vecs_t], [ttk],
                        bias=vecs[0:4, bgo + l * 2 + which:bgo + l * 2 + which + 1])
                    DMA("sp", dst[:, tsl], t[0:4, :], [ttk], [scr_t[dk]])
            pend = [load_w(wv[:, :, 0:512]), load_w(wv[:, :, 512:1024])]
            for pc in range(2):
                w, wt = pend[pc]
                dst_s, dst_t, scale = (qT_s, scr_t["qT"], 1.0) if pc == 0 else (kT_s, scr_t["kT"], SC)
                for tt in range(NTT):
                    tsl = slice(tt * TT, (tt + 1) * TT)
                    for hk in range(4):
                        pt, ptt = next_ps()
                        for k in range(NCH):
                            MM(pt[:], w[:, k, hk * 128:(hk + 1) * 128], yT[:, k, tsl], k == 0, k == NCH - 1,
                               [wt, yT_t[k][tt]], [ptt], inc=(k == NCH - 1))
                        st, stt = next_stg()
                        ACT(st[:, 0:512], pt[:], AF.Copy, [ptt], [stt], scale=scale)
                        DMA("sp", dst_s[hk][:, tsl], st[:, 0:512], [stt], [dst_t])
                if pc == 1:
                    for blk in range(16):
                        bsl = slice(blk * 128, (blk + 1) * 128)
                        pt, ptt = next_ps()
                        for k in range(NCH):
                            MM(pt[:], yT[:, k, bsl], w[:, k, :], k == 0, k == NCH - 1, [wt, yT_t[k][blk // 4]], [ptt],
                               inc=(k == NCH - 1))
                        st, stt = next_stg()
                        ACT(st[:, 0:512], pt[:], AF.Copy, [ptt], [stt], scale=SC)
                        DMA("sp", ktm_s[blk][:, :], st[:, 0:512], [stt], [scr_t["ktm"]])
            for c0, dst_s, dk, func in ((1024, v_s, "v", AF.Copy), (2048, osig_s, "osig", AF.Sigmoid)):
                pend = [load_w(wv[:, :, c0:c0 + 512]), load_w(wv[:, :, c0 + 512:c0 + 1024])]
                for pc in range(2):
                    w, wt = pend[pc]
                    for blk in range(16):
                        bsl = slice(blk * 128, (blk + 1) * 128)
                        pt, ptt = next_ps()
                        for k in range(NCH):
                            MM(pt[:], yT[:, k, bsl], w[:, k, :], k == 0, k == NCH - 1, [wt, yT_t[k][blk // 4]], [ptt],
                               inc=(k == NCH - 1))
                        st, stt = next_stg()
                        ACT(st[:, 0:512], pt[:], func, [ptt], [stt])
                        DMA("sp", dst_s[blk][:, pc * 512:(pc + 1) * 512], st[:, 0:512], [stt], [scr_t[dk]])

            def C_(i):
                return cmp[:, i, :]
            cmpc = cmp[:, 11, :]
            R = [cmp_t]
            DMA("sp", C_(0), gi_s.rearrange("h (c t) -> (h c) t", t=128), [scr_t["gi"]], R)
            DMA("sp", C_(1), gf_s.rearrange("h (c t) -> (h c) t", t=128), [scr_t["gf"]], R)
            ACT(C_(1), C_(1), AF.Exp, R, R, scale=-1.0)
            ACT(C_(1), C_(1), AF.Ln, R, R, bias=1.0)

            def scan(out, d0, d1, init, op0, op1):
                P.op("dve", "tensor_tensor_scan", dict(out=out, data0=d0, data1=d1, initial=init, op0=op0, op1=op1), R, R)

            scan(C_(2), C_(9), C_(1), 0.0, ALU.mult, ALU.subtract)
            TTo("dve", C_(3), C_(0), C_(2), ALU.subtract, R, R)
            CP("dve", cmpc[:, 0:1], cmp[:, 2, 127:128], R, R)
            P.op("dve", "tensor_reduce", dict(out=cmpc[:, 2:3], in_=C_(3), axis=AX.X, op=ALU.max), R, R)
            TTo("dve", cmpc[:, 1:2], cmpc[:, 0:1], cmpc[:, 2:3], ALU.add, R, R)
            DMA("sp", sc1_s[:, :], cmpc[:, 0:2], R, [scr_t["sc1"]])
            rows = cmp[0:4, 12, 0:32]
            DMA("sp", rows, sc1_s.rearrange("(h c) two -> h (c two)", c=16), [scr_t["sc1"]], R)
            rows3 = rows.rearrange("p (c two) -> p c two", two=2)
            Gr = cmp[0:4, 12, 32:48]; GBr = cmp[0:4, 12, 48:64]; mn = cmp[0:4, 12, 64:80]; ms = cmp[0:4, 12, 80:96]
            CP("dve", Gr, rows3[:, :, 0], R, R)
            CP("dve", GBr, rows3[:, :, 1], R, R)
            scan(mn, Gr, GBr, 0.0, ALU.add, ALU.max)
            P.op("dve", "memset", dict(ap=ms[:, 0:1], constant=0.0), R, R)
            CP("dve", ms[:, 1:16], mn[:, 0:15], R, R)
            DMA("sp", sc2_s[:, :], ms, R, [scr_t["sc2"]])
            DMA("sp", cmpc[:, 3:4], bass.AP(sc2_s.tensor, 0, [[1, 64], [1, 1]]), [scr_t["sc2"]], R)
            scan(C_(4), C_(10), C_(3), cmpc[:, 3:4], ALU.add, ALU.max)
            TS("dve", C_(5), C_(4), -1.0, None, ALU.mult, ALU.bypass, R, R)
            ACT(C_(6), C_(4), AF.Exp, R, R, scale=-1.0, bias=cmpc[:, 3:4])
            TTo("dve", C_(7), C_(2), C_(4), ALU.add, R, R)
            ACT(C_(7), C_(7), AF.Exp, R, R, scale=-1.0)
            TS("dve", cmpc[:, 4:5], cmp[:, 4, 127:128], -1.0, None, ALU.mult, ALU.bypass, R, R)
            ACT(C_(8), C_(3), AF.Exp, R, R, bias=cmpc[:, 4:5])
            DMA("sp", negu_s[:, :], C_(5), R, [scr_t["negu"]])
            DMA("sp", wint_s[:, :], C_(6), R, [scr_t["wint"]])
            DMA("sp", wold_s[:, :], cmp[:, 6, 127:128], R, [scr_t["wold"]])
            pt, ptt = next_ps()
            for qi, slot in enumerate((3, 7, 8)):
                P.op("pe", "transpose", dict(out=pt[:, qi * 64:(qi + 1) * 64], in_=C_(slot), identity=consts[0:64, 0:64]),
                     R + [consts_t], [ptt], inc=True)
            CP("dve", cols3[:, 0:3, :], pt[:, 0:192].rearrange("p (q j) -> p q j", q=3), [ptt], [cols3_t])
            DMA("sp", cols3[:, 3, :], bass.AP(wold_s.tensor, 0, [[0, 128], [1, 64]]), [scr_t["wold"]], [cols3_t])

            prefetched[("op", b, l)] = [load_w(ml_w_out[l].rearrange("(k p) n -> p k n", p=128)[:, :, 0:512]),
                                        load_w(ml_w_out[l].rearrange("(k p) n -> p k n", p=128)[:, :, 512:1024])]
            Vc = [mxv[:, p * 1028:(p + 1) * 1028].rearrange("p (h v) -> p h v", h=4) for p in range(2)]
            for p in range(2):
                P.op("pool", "memset", dict(ap=Vc[p][:, :, 256:257], constant=1.0), [], [mxv_t[p]])
            numb = obuf[:, 0:1028].rearrange("p (h v) -> p h v", h=4)
            hgo = vec_cols[f"hg{l}"][0]
            pending_tail = [None]
            for c in range(16):
                p = c % 2
                tt = c // 4
                csl = slice(c * 128, (c + 1) * 128)
                qc = mxa[:, p * 1152:p * 1152 + 512].rearrange("p (h t) -> p h t", h=4)
                kc = mxa[:, p * 1152 + 512:p * 1152 + 1024].rearrange("p (h t) -> p h t", h=4)
                DMA("sp", qc, bass.AP(qT_s.tensor, c * 128, [[S, 128], [128 * S, 4], [1, 128]]), [scr_t["qT"]], [mxa_t[p]])
                DMA("sp", kc, bass.AP(kT_s.tensor, c * 128, [[S, 128], [128 * S, 4], [1, 128]]), [scr_t["kT"]], [mxa_t[p]])
                DMA("sp", mxq[p][:, :], ktm_s[c][:, :], [scr_t["ktm"]], [mxq_t[p]])
                DMA("sp", Vc[p][:, :, 0:256], v_s[c].rearrange("p (h v) -> p h v", h=4), [scr_t["v"]], [mxv_t[p]])
                og, ogt = next_stg()
                DMA("sp", og[:, :], osig_s[c][:, :], [scr_t["osig"]], [ogt])
                DMA("sp", nw4[:, 0], bass.AP(negu_s.tensor, c * 128, [[0, 128], [16 * 128, 4], [1, 128]]),
                    [scr_t["negu"]], [nw4_t])
                DMA("sp", nw4[:, 1], bass.AP(wint_s.tensor, c * 128, [[0, 128], [16 * 128, 4], [1, 128]]),
                    [scr_t["wint"]], [nw4_t])
                hd = []
                sps = []
                for h in range(4):
                    pts, ptst = next_ps()
                    MM(pts[:, 0:128], kc[:, h, :], qc[:, h, :], True, True, [mxa_t[p]], [ptst], inc=True)
                    sps.append((pts, ptst))
                for h in range(4):
                    j = h * 16 + c
                    wt_, wtt = next_tmp()
                    ACT(wt_[:, 0:128], nw4[:, 0, h, :], AF.Exp, [nw4_t, cols3_t], [wtt], bias=cols3[:, 0, j:j + 1])
                    TTo("pool", wt_[:, 0:128], wt_[:, 0:128], trif, ALU.mult, [wtt, consts_t], [wtt])
                    ei = cnt["e"] % 4
                    cnt["e"] += 1
                    sw = ebuf[ei][:, 0:128]; qs_ = ebuf[ei][:, 128:256]; ks_ = ebuf[ei][:, 256:384]
                    et = ebuf_t[ei]
                    TTo("dve", sw, sps[h][0][:, 0:128], wt_[:, 0:128], ALU.mult, [sps[h][1], wtt], [et])
                    TTo("pool", qs_, qc[:, h, :], nw4[:, 1, h, :], ALU.mult, [mxa_t[p], nw4_t], [et])
                    ACT(ks_, mxq[p][:, h * 128:(h + 1) * 128], AF.Copy, [mxq_t[p], cols3_t], [et],
                        scale=cols3[:, 2, j:j + 1])
                    hd.append((sw, qs_, ks_, et))
                for h in range(4):
                    sw, qs_, ks_, et = hd[h]
                    pn, pnt = next_ps()
                    if c > 0:
                        MM(pn[:, 0:257], qs_, cstb[:, h, :], True, False, [et, cstb_t[h]], [pnt], inc=True)
                    MM(pn[:, 0:257], sw, Vc[p][:, h, :], c == 0, True, [et, mxv_t[p]], [pnt], inc=True)
                    ACT(numb[:, h, :], pn[:, 0:257], AF.Copy, [pnt], [obuf_t])
                if c < 15:
                    for h in range(4):
                        j = h * 16 + c
                        sw, qs_, ks_, et = hd[h]
                        pc_, pct = next_ps()
                        MM(pc_[:, 0:257], ks_, Vc[p][:, h, :], True, True, [et, mxv_t[p]], [pct], inc=True)
                        if c == 0:
                            CP("dve", cst[:, h, 0:257], pc_[:, 0:257], [pct], [cst_t[h]])
                        else:
                            STT(cst[:, h, 0:257], cst[:, h, 0:257], cols3[:, 3, j:j + 1], pc_[:, 0:257], ALU.mult, ALU.add,
                                [cst_t[h], cols3_t, pct], [cst_t[h]])
                        CP("pool", cstb[:, h, :], cst[:, h, 0:257], [cst_t[h]], [cstb_t[h]])
                if pending_tail[0] is not None:
                    pending_tail[0]()
                    pending_tail[0] = None
                dd = small[:, 16:20]; ssq = small[:, 20:24]; t1 = small[:, 24:28]; var = small[:, 28:32]; rr = small[:, 32:36]
                SR = [small_t]
                STT(dd, numb[:, :, 256], -1.0, numb[:, :, 256], ALU.mult, ALU.max, [obuf_t], SR)
                emr_c = bass.AP(cols3, 64 + c, [[256, 128], [16, 4]])
                TTo("dve", dd, dd, emr_c, ALU.max, SR + [cols3_t], SR)
                for h in range(4):
                    sqt, sqtt = next_tmp()
                    TTo("pool", sqt[:, 0:256], numb[:, h, 0:256], numb[:, h, 0:256], ALU.mult, [obuf_t], [sqtt])
                    P.op("dve", "tensor_reduce", dict(out=ssq[:, h:h + 1], in_=sqt[:, 0:256], axis=AX.X, op=ALU.add),
                         [sqtt], SR)
                STT(t1, dd, 1e-6, dd, ALU.mult, ALU.mult, SR, SR)
                STT(var, ssq, 1.0 / 256, t1, ALU.mult, ALU.add, SR, SR)
                ACT(var, var, AF.Ln, SR, SR)
                ACT(rr, var, AF.Exp, SR, SR, scale=-0.5)
                hg, hgt = next_stg()
                for h in range(4):
                    STT(hg[:, h * 256:(h + 1) * 256], numb[:, h, 0:256], rr[:, h:h + 1], og[:, h * 256:(h + 1) * 256],
                        ALU.mult, ALU.mult, [obuf_t, ogt] + SR, [hgt])

                def tail(hg=hg, hgt=hgt, csl=csl, tt=tt):
                    ptp, ptpt = next_ps()
                    tp = ptp.bitcast(BF16)
                    for jv in range(8):
                        P.op("pe", "transpose", dict(out=tp[:, jv * 128:(jv + 1) * 128], in_=hg[:, jv * 128:(jv + 1) * 128],
                                                     identity=ident_bf), [hgt, consts_t], [ptpt], inc=(jv == 7))
                    for jv in range(8):
                        ACT(yT[:, jv, csl], tp[:, jv * 128:(jv + 1) * 128], AF.Copy, [ptpt, vecs_t], [yT_t[jv][tt]],
                            scale=vecs[:, hgo + jv:hgo + jv + 1])

                pending_tail[0] = tail
            pending_tail[0]()
            pending_tail[0] = None
            out_proj(b, l, ml_w_out[l])

        tri_bf = cbf[:, 256:384]
        CP("pool", tri_bf, trif, [consts_t], [consts_t])
        for b in range(nseq):
            cur["b"] = b
            for k in range(NCH):
                DMA("sp", xT[:, k, :], x_t[b].rearrange("(k p) t -> p k t", p=128)[:, k, :], [],
                    [xT_t[k][t] for t in range(NTT)])
            if do_attn:
                rope_tables(b)
            for l in range(DEPTH):
                if l < 2 and do_mlstm:
                    mlstm_mixer(b, l)
                if l >= 2 and do_attn and not (debug and l == 3):
                    attn_mixer(b, l)
                mlp_prefetch(b, l)
                norm_to_y(b, gsc[l][1], modT[l], 3 * NCH)
                mlp(b, l)
                if l == 1 and do_attn:
                    kv_stage(b)
            fo = vec_cols["fin"][0]
            for tt in range(NTT):
                tsl = slice(tt * TT, (tt + 1) * TT)

                def out_fn(k, xr, xrt, tt=tt, tsl=tsl, b=b):
                    ot, ott = next_tmp()
                    ACT(ot[:], xr[:], AF.Copy, [xrt, vecs_t], [ott], scale=vecs[:, fo + k:fo + k + 1])
                    DMA("sp", out_t[b].rearrange("(k p) t -> p k t", p=128)[:, k, tsl], ot[:], [ott], [], sem="d_out")

                norm_tile(tt, out_fn)

        P.barrier()
        sems = {k: es.enter_context(nc.semaphore(f"s_{k}")) for k in P.cnt.keys()}
        with nc.Block() as block:
            P.emit(nc, block, sems)
    return nc


W_NAMES = ["ada_w", "kv_ada_w", "mlp_w_up", "mlp_w_down", "mlstm_w_in", "mlstm_w_out", "mlstm_head_g", "kv_w",
           "diff_w_q", "diff_w_out"]


def prep_inputs(inp, n_cores, nseq):
    vecs, cols = pack_vecs(inp)
    consts = make_consts()
    x = np.asarray(inp["x"], dtype=np.float32)
    c = np.asarray(inp["c"], dtype=np.float32)
    pos = np.asarray(inp["positions"], dtype=np.int32)
    shared = {n: np.ascontiguousarray(np.asarray(inp[n], dtype=np.float32)) for n in W_NAMES}
    shared["diff_lambda"] = np.ascontiguousarray(np.asarray(inp["diff_lambda"], dtype=np.float32).reshape(2, 256))
    shared["diff_subln_g"] = np.ascontiguousarray(np.asarray(inp["diff_subln_g"], dtype=np.float32))
    maps = []
    for i in range(n_cores):
        sl = slice(i * nseq, (i + 1) * nseq)
        m = dict(shared)
        m["x_t"] = np.ascontiguousarray(x[sl].transpose(0, 2, 1))
        m["c_t"] = np.ascontiguousarray(c[sl].T.reshape(NCH, 128, -1).transpose(1, 0, 2))
        m["positions"] = np.ascontiguousarray(pos[sl])
        m["vecs"] = vecs
        m["consts"] = consts
        maps.append(m)
    return maps, cols, vecs.shape[1]


def kernel(**inputs):
    n_cores = 8
    B = inputs["x"].shape[0]
    nseq = B // n_cores
    maps, cols, nvec = prep_inputs(inputs, n_cores, nseq)
    nc = build(nseq, cols, nvec)
    res = run_bass_kernel_spmd(nc, maps, core_ids=list(range(n_cores)))
    outs = [np.asarray(r["out_t"]) for r in res.results]
    out = np.concatenate(outs, axis=0).transpose(0, 2, 1)
    return np.ascontiguousarray(out.astype(np.float32))
```
